# Optimizing a Trainium2 kernel written in Bass

```python
import math, functools
import jax, jax.numpy as jnp
from jax import lax
import numpy as np

D_MODEL = 4096
BATCH = 2
SEQ = 8192
DEPTH = 2

GRID_W = 64
CTX_LEN = 256
D_MIX = D_MODEL
D_SSM = D_MIX // 2
D_CONF = D_MIX - D_SSM
SSM_HEADDIM = 64
SSM_HEADS = D_SSM // SSM_HEADDIM
SSM_GROUPS = 8
HEADS_PER_GROUP = SSM_HEADS // SSM_GROUPS
SSM_STATE = 128
SSM_CONV = 5
CHUNK = 128
D_XB = D_SSM + SSM_GROUPS * SSM_STATE
D_XBC = D_XB + SSM_GROUPS * SSM_STATE
DT_MIN = 1e-3
DT_MAX = 1e-1
A_MIN = 1.0
A_MAX = 16.0
CONF_WIDTH = 31
OFF_DT = D_SSM
OFF_X = OFF_DT + 2 * SSM_HEADS
OFF_C = OFF_X + D_XB
OFF_GLU = OFF_X + D_XBC
D_IN_PROJ = OFF_GLU + 2 * D_CONF
FFN_DENSE = 11008
N_EXPERTS = 8
TOP_K = 2
FFN_EXPERT = 3584
LN_EPS = 1e-5
ALPHA = (2 * DEPTH) ** 0.25
BETA = (8 * DEPTH) ** -0.25

kernel_name = "hybrid_ssd_conformer_moe_dit"


def _standardize(t):
    tf = t.astype(jnp.float32)
    mu = jnp.mean(tf, axis=-1, keepdims=True)
    var = jnp.mean(jnp.square(tf - mu), axis=-1, keepdims=True)
    return (tf - mu) * lax.rsqrt(var + LN_EPS)


def layer_norm(t, gain, bias):
    return (_standardize(t) * gain + bias).astype(t.dtype)


def modulate(t, shift, scale):
    return (_standardize(t) * (1 + scale) + shift).astype(t.dtype)


def dwconv1d(v, w, b):
    out = lax.conv_general_dilated(v, w[:, None, :], window_strides=(1,), padding="SAME",
                                   dimension_numbers=("NWC", "WIO", "NWC"),
                                   feature_group_count=v.shape[-1])
    return out + b


def dwconv_grid(v, w, b, rows, vertical):
    bsz, length, ch = v.shape
    img = v.reshape(bsz, rows, GRID_W, ch)
    taps = w[:, None, None, :] if vertical else w[None, :, None, :]
    out = lax.conv_general_dilated(img, taps, window_strides=(1, 1), padding="SAME",
                                   dimension_numbers=("NHWC", "HWIO", "NHWC"),
                                   feature_group_count=ch)
    return out.reshape(bsz, length, ch) + b


def ssm_conv(xbc_raw, w, b):
    return jax.nn.silu(dwconv1d(xbc_raw, w, b))


def flip(t):
    return jnp.flip(t, axis=1)


def to_chunks(t):
    return t.reshape(t.shape[0], t.shape[1] // CHUNK, CHUNK, *t.shape[2:])


def ssd_direction(xs, dt_raw, dt_bias, a_log):
    bsz, length = dt_raw.shape[:2]
    dt = jax.nn.softplus(dt_raw.astype(jnp.float32) + dt_bias.astype(jnp.float32))
    dt = dt.reshape(bsz, length, SSM_GROUPS, HEADS_PER_GROUP)
    a = -dt * jnp.exp(a_log.astype(jnp.float32)).reshape(SSM_GROUPS, HEADS_PER_GROUP)
    return xs * dt[..., None].astype(xs.dtype), a


def ssd_states(xc, ac, bc, init):
    a_cum = jnp.cumsum(ac, axis=2)
    a_tot = a_cum[:, :, -1]
    decay_to_end = jnp.exp(a_tot[:, :, None] - a_cum).astype(xc.dtype)
    chunk_states = jnp.einsum("bclgn,bclge,bclgep->bcgepn", bc, decay_to_end, xc)
    chunk_decay = jnp.exp(a_tot).astype(xc.dtype)

    def step(h, inp):
        s, d = inp
        return h * d[..., None, None] + s, h

    final, prev = lax.scan(step, init, (jnp.moveaxis(chunk_states, 1, 0),
                                        jnp.moveaxis(chunk_decay, 1, 0)))
    return a_cum, jnp.moveaxis(prev, 0, 1), final


def ssd_output(xc, bc, cc, a_cum, prev):
    seg = a_cum[:, :, :, None] - a_cum[:, :, None, :]
    lower = jnp.tril(jnp.ones((CHUNK, CHUNK), dtype=bool))[:, :, None, None]
    decay = jnp.exp(jnp.where(lower, seg, -jnp.inf)).astype(xc.dtype)
    cb = jnp.einsum("bclgn,bcsgn->bclsg", cc, bc)
    y_diag = jnp.einsum("bclsge,bcsgep->bclgep", cb[..., None] * decay, xc)
    y_off = jnp.einsum("bclgn,bcgepn->bclgep", cc, prev) * jnp.exp(a_cum).astype(xc.dtype)[..., None]
    return y_diag + y_off


def ssd_scan(xdt, a, bm, cm, init):
    xc, bc, cc = to_chunks(xdt), to_chunks(bm), to_chunks(cm)
    a_cum, prev, final = ssd_states(xc, to_chunks(a), bc, init)
    y = ssd_output(xc, bc, cc, a_cum, prev)
    return y.reshape(xdt.shape), final


def ssd_final(xdt, a, bm, init):
    return ssd_states(to_chunks(xdt), to_chunks(a), to_chunks(bm), init)[2]


def gated_rmsnorm(y, z, w):
    g = (y * jax.nn.silu(z)).astype(jnp.float32)
    g = g.reshape(*g.shape[:-1], SSM_GROUPS, D_SSM // SSM_GROUPS)
    g = g * lax.rsqrt(jnp.mean(jnp.square(g), axis=-1, keepdims=True) + LN_EPS)
    return (g.reshape(y.shape) * w).astype(y.dtype)


def split_heads(xb_or_xbc, bsz, length):
    xs = xb_or_xbc[..., :D_SSM].reshape(bsz, length, SSM_GROUPS, HEADS_PER_GROUP, SSM_HEADDIM)
    bm = xb_or_xbc[..., D_SSM:D_XB].reshape(bsz, length, SSM_GROUPS, SSM_STATE)
    return xs, bm


def token_mixer(u, p, init_f, init_b, conf_conv):
    bsz, length = u.shape[:2]
    proj = u @ p["w_in"]
    z = proj[..., :OFF_DT]
    dt_raw = proj[..., OFF_DT:OFF_X]
    xbc = ssm_conv(proj[..., OFF_X:OFF_GLU], p["conv_w"], p["conv_b"])
    glu = proj[..., OFF_GLU:]
    xs, bm = split_heads(xbc, bsz, length)
    cm = xbc[..., D_XB:].reshape(bsz, length, SSM_GROUPS, SSM_STATE)
    xdt_f, a_f = ssd_direction(xs, dt_raw[..., :SSM_HEADS], p["dt_bias_f"], p["a_log_f"])
    xdt_b, a_b = ssd_direction(xs, dt_raw[..., SSM_HEADS:], p["dt_bias_b"], p["a_log_b"])
    y_f, fin_f = ssd_scan(xdt_f, a_f, bm, cm, init_f)
    y_b, fin_b = ssd_scan(flip(xdt_b), flip(a_b), flip(bm), flip(cm), init_b)
    y = y_f + flip(y_b) + xs * p["d_skip"].reshape(SSM_GROUPS, HEADS_PER_GROUP, 1)
    y_ssm = gated_rmsnorm(y.reshape(bsz, length, D_SSM), z, p["ssm_norm_w"])
    val, gate = jnp.split(glu, 2, axis=-1)
    v = conf_conv(val * jax.nn.sigmoid(gate), p["conf_conv_w"], p["conf_conv_b"])
    v = jax.nn.silu(layer_norm(v, p["conf_ln_g"], p["conf_ln_b"]))
    out = jnp.concatenate([y_ssm, v], axis=-1) @ p["w_out"]
    return out, fin_f, fin_b


def context_final_states(u, p, init):
    bsz, length = u.shape[:2]
    cols = u @ p["w_in"][:, OFF_DT:OFF_C]
    dt_raw = cols[..., :2 * SSM_HEADS]
    xb = ssm_conv(cols[..., 2 * SSM_HEADS:], p["conv_w"][:, :D_XB], p["conv_b"][:D_XB])
    xs, bm = split_heads(xb, bsz, length)
    xdt_f, a_f = ssd_direction(xs, dt_raw[..., :SSM_HEADS], p["dt_bias_f"], p["a_log_f"])
    xdt_b, a_b = ssd_direction(xs, dt_raw[..., SSM_HEADS:], p["dt_bias_b"], p["a_log_b"])
    fin_f = ssd_final(xdt_f, a_f, bm, init)
    fin_b = ssd_final(flip(xdt_b), flip(a_b), flip(bm), init)
    return fin_f, fin_b


def swiglu(u, w1, w3, w2):
    return (jax.nn.silu(u @ w1) * (u @ w3)) @ w2


def moe_swiglu(u, router_w, router_b, w1, w3, w2):
    logits = (u @ router_w + router_b).astype(jnp.float32)
    top_val, top_idx = lax.top_k(logits, TOP_K)
    top_w = jax.nn.softmax(top_val, axis=-1)
    gates = jnp.sum(jax.nn.one_hot(top_idx, N_EXPERTS, dtype=jnp.float32) * top_w[..., None],
                    axis=-2).astype(u.dtype)
    out = jnp.zeros_like(u)
    for e in range(N_EXPERTS):
        out = out + gates[..., e:e + 1] * swiglu(u, w1[e], w3[e], w2[e])
    return out


def setup_inputs(seed: int = 0) -> dict:
    key = jax.random.key(seed)
    keys = iter(jax.random.split(key, 64))

    def normal(shape, scale):
        return jax.random.normal(next(keys), shape, jnp.float32) * scale

    def dt_bias(shape):
        dt = jnp.exp(jax.random.uniform(next(keys), shape, jnp.float32,
                                        math.log(DT_MIN), math.log(DT_MAX)))
        return dt + jnp.log(-jnp.expm1(-dt))

    def a_log(shape):
        return jnp.log(jax.random.uniform(next(keys), shape, jnp.float32, A_MIN, A_MAX))

    n_dense = (DEPTH + 1) // 2
    n_moe = DEPTH // 2
    return {
        "x": normal((BATCH, SEQ, D_MODEL), 1.0),
        "c": normal((BATCH, D_MODEL), 1.0),
        "ctx": normal((BATCH, CTX_LEN, D_MODEL), 1.0),
        "c_ctx": normal((D_MODEL,), 1.0),
        "ada_w": normal((DEPTH, D_MODEL, 6 * D_MODEL), 0.5 * D_MODEL ** -0.5),
        "ada_b": normal((DEPTH, 6 * D_MODEL), 0.02),
        "w_in": normal((DEPTH, D_MODEL, D_IN_PROJ), D_MODEL ** -0.5),
        "mamba_conv_w": normal((DEPTH, SSM_CONV, D_XBC), SSM_CONV ** -0.5),
        "mamba_conv_b": normal((DEPTH, D_XBC), 0.02),
        "dt_bias_fwd": dt_bias((DEPTH, SSM_HEADS)),
        "dt_bias_bwd": dt_bias((DEPTH, SSM_HEADS)),
        "a_log_fwd": a_log((DEPTH, SSM_HEADS)),
        "a_log_bwd": a_log((DEPTH, SSM_HEADS)),
        "d_skip": 1.0 + normal((DEPTH, SSM_HEADS), 0.1),
        "ssm_norm_w": 1.0 + normal((DEPTH, D_SSM), 0.02),
        "conf_conv_w": normal((DEPTH, CONF_WIDTH, D_CONF), CONF_WIDTH ** -0.5),
        "conf_conv_b": normal((DEPTH, D_CONF), 0.02),
        "conf_ln_g": 1.0 + normal((DEPTH, D_CONF), 0.02),
        "conf_ln_b": normal((DEPTH, D_CONF), 0.02),
        "w_out": normal((DEPTH, D_MIX, D_MODEL), BETA * D_MIX ** -0.5),
        "ln1_g": 1.0 + normal((DEPTH, D_MODEL), 0.02),
        "ln1_b": normal((DEPTH, D_MODEL), 0.02),
        "ln2_g": 1.0 + normal((DEPTH, D_MODEL), 0.02),
        "ln2_b": normal((DEPTH, D_MODEL), 0.02),
        "ffn_w1": normal((n_dense, D_MODEL, FFN_DENSE), D_MODEL ** -0.5),
        "ffn_w3": normal((n_dense, D_MODEL, FFN_DENSE), D_MODEL ** -0.5),
        "ffn_w2": normal((n_dense, FFN_DENSE, D_MODEL), BETA * FFN_DENSE ** -0.5),
        "router_w": normal((n_moe, D_MODEL, N_EXPERTS), D_MODEL ** -0.5),
        "router_b": normal((n_moe, N_EXPERTS), 0.01),
        "moe_w1": normal((n_moe, N_EXPERTS, D_MODEL, FFN_EXPERT), D_MODEL ** -0.5),
        "moe_w3": normal((n_moe, N_EXPERTS, D_MODEL, FFN_EXPERT), D_MODEL ** -0.5),
        "moe_w2": normal((n_moe, N_EXPERTS, FFN_EXPERT, D_MODEL), BETA * FFN_EXPERT ** -0.5),
    }


def reference(x, c, ctx, c_ctx, ada_w, ada_b, w_in, mamba_conv_w, mamba_conv_b,
              dt_bias_fwd, dt_bias_bwd, a_log_fwd, a_log_bwd, d_skip, ssm_norm_w,
              conf_conv_w, conf_conv_b, conf_ln_g, conf_ln_b, w_out,
              ln1_g, ln1_b, ln2_g, ln2_b, ffn_w1, ffn_w3, ffn_w2,
              router_w, router_b, moe_w1, moe_w3, moe_w2):
    bsz, seq, _ = x.shape
    rows = seq // GRID_W
    cond = jax.nn.silu(c)
    cond_ctx = jax.nn.silu(c_ctx)
    zero_state = jnp.zeros((bsz, SSM_GROUPS, HEADS_PER_GROUP, SSM_HEADDIM, SSM_STATE), x.dtype)

    def channel_mixer(u, i):
        j = i // 2
        if i % 2 == 0:
            return swiglu(u, ffn_w1[j], ffn_w3[j], ffn_w2[j])
        return moe_swiglu(u, router_w[j], router_b[j], moe_w1[j], moe_w3[j], moe_w2[j])

    h, h_ctx = x, ctx
    for i in range(DEPTH):
        p = {"w_in": w_in[i], "conv_w": mamba_conv_w[i], "conv_b": mamba_conv_b[i],
             "dt_bias_f": dt_bias_fwd[i], "dt_bias_b": dt_bias_bwd[i],
             "a_log_f": a_log_fwd[i], "a_log_b": a_log_bwd[i], "d_skip": d_skip[i],
             "ssm_norm_w": ssm_norm_w[i], "conf_conv_w": conf_conv_w[i],
             "conf_conv_b": conf_conv_b[i], "conf_ln_g": conf_ln_g[i],
             "conf_ln_b": conf_ln_b[i], "w_out": w_out[i]}
        mods = jnp.split(cond @ ada_w[i] + ada_b[i], 6, axis=-1)
        sh1, sc1, g1, sh2, sc2, g2 = [m[:, None, :] for m in mods]
        csh1, csc1, cg1, csh2, csc2, cg2 = jnp.split(cond_ctx @ ada_w[i] + ada_b[i], 6)

        u_ctx = modulate(h_ctx, csh1, csc1)
        if i == DEPTH - 1:
            fin_f, fin_b = context_final_states(u_ctx, p, zero_state)
        else:
            mix_ctx, fin_f, fin_b = token_mixer(u_ctx, p, zero_state, zero_state, dwconv1d)
            h_ctx = layer_norm(ALPHA * h_ctx + cg1 * mix_ctx, ln1_g[i], ln1_b[i])
            h_ctx = layer_norm(ALPHA * h_ctx + cg2 * channel_mixer(modulate(h_ctx, csh2, csc2), i),
                               ln2_g[i], ln2_b[i])

        grid_conv = functools.partial(dwconv_grid, rows=rows, vertical=(i % 2 == 1))
        mix, _, _ = token_mixer(modulate(h, sh1, sc1), p, fin_f, fin_b, grid_conv)
        h = layer_norm(ALPHA * h + g1 * mix, ln1_g[i], ln1_b[i])
        h = layer_norm(ALPHA * h + g2 * channel_mixer(modulate(h, sh2, sc2), i), ln2_g[i], ln2_b[i])
    return h
```

```python
import numpy as np
import concourse.bass as bass
import concourse.mybir as mybir
from concourse.bass_utils import run_bass_kernel_spmd

N_CORES_USED = 2

F32 = mybir.dt.float32
BF16 = mybir.dt.bfloat16
AF = mybir.ActivationFunctionType
ALU = mybir.AluOpType
AX = mybir.AxisListType


class Buf:
    def __init__(self, k, name, t):
        self.k = k
        self.name = name
        self.t = t
        self.w = {}
        self.r = {}
        self.sem = None
        self.cnt = 0
        k.allbufs.append(self)

    def __getitem__(self, idx):
        return self.t[idx]

    def rearrange(self, *a, **kw):
        return self.t.rearrange(*a, **kw)

    def dsem(self):
        if self.sem is None:
            if self.k.free_sems:
                self.sem, self.cnt = self.k.free_sems.pop()
            else:
                self.sem = self.k.new_sem("d%d" % len(self.k._semctx))
        return self.sem


class K:
    ENG = ("pe", "act", "dve", "pool", "sp")

    def __init__(self, nc):
        self.nc = nc
        self.eng = {"pe": nc.tensor, "act": nc.scalar, "dve": nc.vector, "pool": nc.gpsimd, "sp": nc.sync}
        self._semctx = []
        self._ctx = []
        self.allbufs = []
        self.free_sems = []
        self.esem = {e: self.new_sem("e_" + e) for e in ("pe", "act", "dve", "pool")}
        self.ecnt = {e: 0 for e in self.esem}
        self.seen = {e: {} for e in self.ENG}
        self.nwait = 0
        self.nops = 0
        self.self_wait = {"pe": False, "act": True, "dve": True, "pool": True, "sp": True}

    def new_sem(self, name):
        cm = self.nc.semaphore(name)
        s = cm.__enter__()
        self._semctx.append(cm)
        return s

    def uniq(self, name):
        self._n = getattr(self, "_n", 0) + 1
        return "%s_%d" % (name, self._n)

    def sbuf(self, name, shape, dt):
        name = self.uniq(name)
        cm = self.nc.sbuf_tensor(name, list(shape), dt)
        t = cm.__enter__()
        b = Buf(self, name, t)
        self._ctx.append((cm, b))
        return b

    def psum(self, name, shape, dt=F32):
        name = self.uniq(name)
        cm = self.nc.psum_tensor(name, list(shape), dt)
        t = cm.__enter__()
        b = Buf(self, name, t)
        self._ctx.append((cm, b))
        return b

    def dram(self, name, shape, dt, kind="Internal"):
        t = self.nc.dram_tensor(name, list(shape), dt, kind=kind)
        return Buf(self, name, t.ap())

    def region(self, name):
        return Buf(self, name, None)

    def _need(self, reads, writes, parts):
        need = {}

        def add(d):
            for sid, (s, v) in d.items():
                if sid not in need or need[sid][1] < v:
                    need[sid] = (s, v)

        for b in reads:
            add(b.w)
        for b in writes:
            add(b.w)
            add(b.r)
        for b in parts:
            add(b.r)
        return need

    def _emit_waits(self, e, need):
        seen = self.seen[e]
        eng = self.eng[e]
        own = self.esem.get(e)
        for sid, (s, v) in need.items():
            if seen.get(sid, 0) >= v:
                continue
            if s is own and not self.self_wait[e]:
                continue
            eng.wait_ge(s, v)
            self.nwait += 1
            seen[sid] = v

    def _record(self, tok, reads, writes, parts):
        sid = id(tok[0])
        for b in reads:
            if sid not in b.r or b.r[sid][1] < tok[1]:
                b.r[sid] = tok
        for b in writes:
            b.w = {sid: tok}
            b.r = {}
        for b in parts:
            if sid not in b.w or b.w[sid][1] < tok[1]:
                b.w[sid] = tok

    def op(self, e, fn, reads=(), writes=(), parts=()):
        need = self._need(reads, writes, parts)
        self._emit_waits(e, need)
        ins = fn(self.eng[e])
        self.ecnt[e] += 1
        self.nops += 1
        ins.then_inc(self.esem[e], 1)
        tok = (self.esem[e], self.ecnt[e])
        self._record(tok, reads, writes, parts)
        return tok

    def dma(self, q, out, in_, sb, reads=(), writes=(), parts=(), **kw):
        need = self._need(reads, writes, parts)
        self._emit_waits(q, need)
        ins = self.eng[q].dma_start(out=out, in_=in_, **kw)
        s = sb.dsem()
        sb.cnt += 16
        ins.then_inc(s, 16)
        tok = (s, sb.cnt)
        self._record(tok, reads, writes, parts)
        return tok

    def finish(self, bufs):
        need = {}
        for b in bufs:
            for d in (b.w, b.r):
                for sid, (s, v) in d.items():
                    if sid not in need or need[sid][1] < v:
                        need[sid] = (s, v)
        self._emit_waits("sp", need)

    def mark(self):
        return len(self._ctx)

    def barrier(self):
        need = {}
        for e, s in self.esem.items():
            if self.ecnt[e]:
                need[id(s)] = (s, self.ecnt[e])
        for b in self.allbufs:
            if b.sem is not None and b.cnt:
                need[id(b.sem)] = (b.sem, b.cnt)
        for e in self.ENG:
            self._emit_waits(e, dict(need))

    def release(self, mark):
        self.barrier()
        while len(self._ctx) > mark:
            cm, b = self._ctx.pop()
            if b.sem is not None:
                self.free_sems.append((b.sem, b.cnt))
                b.sem = None
            cm.__exit__(None, None, None)

    def close(self):
        self.release(0)
        for cm in reversed(self._semctx):
            cm.__exit__(None, None, None)
        self._semctx = []


class Cfg:
    def __init__(self, D=4096, G=8, E=4, LC=256, L=8192, GW=64, FD=11008, NE=8, FE=3584, depth=2,
                 TB=1024, NSUB=256):
        self.D = D
        self.KC = D // 128
        self.G, self.E, self.P, self.N = G, E, 64, 128
        self.H = G * E
        self.DS = self.H * 64
        self.DCF = D - self.DS
        self.DXB = self.DS + G * 128
        self.DXBC = self.DXB + G * 128
        self.OFF_DT = self.DS
        self.OFF_X = self.OFF_DT + 2 * self.H
        self.OFF_GLU = self.OFF_X + self.DXBC
        self.NIN = self.OFF_GLU + 2 * self.DCF
        self.LC, self.L, self.T = LC, L, LC + L
        self.GW = GW
        self.KS, self.KW = 5, 31
        self.FD, self.NE, self.FE = FD, NE, FE
        self.depth = depth
        self.ALPHA = (2 * depth) ** 0.25
        self.EPS = 1e-5
        self.TB, self.NSUB = TB, NSUB


def tblocks(c, tb=None):
    tb = tb or c.TB
    out = [(0, c.LC)]
    t = c.LC
    while t < c.T:
        n = min(tb, c.T - t)
        out.append((t, n))
        t += n
    return out


def nsplit(n, w=512):
    return [(o, min(w, n - o)) for o in range(0, n, w)]


class MK:
    def __init__(self, nc, cfg, dbg=None):
        self.nc, self.c = nc, cfg
        self.k = K(nc)
        self.dbg = dbg or {}
        k, c = self.k, cfg
        self.I = {}
        self.evac_rr = 0

        def inp(name, shape):
            self.I[name] = k.dram(name, shape, F32, kind="ExternalInput")

        dp = c.depth
        nd, nm = (dp + 1) // 2, dp // 2
        inp("x", [c.L, c.D]); inp("ctx", [c.LC, c.D]); inp("c2", [2, c.D])
        inp("ada_w", [dp, c.D, 6 * c.D]); inp("ada_b", [dp, 6 * c.D]); inp("w_in", [dp, c.D, c.NIN])
        inp("conv_w", [dp, c.KS, c.DXBC]); inp("conv_b", [dp, c.DXBC])
        for n_ in ("dtb_f", "dtb_b", "alog_f", "alog_b", "dskip"):
            inp(n_, [dp, c.H])
        inp("ssm_norm_w", [dp, c.DS]); inp("cconv_w", [dp, c.KW, c.DCF])
        for n_ in ("cconv_b", "cln_g", "cln_b"):
            inp(n_, [dp, c.DCF])
        inp("w_out", [dp, c.D, c.D])
        for n_ in ("ln1_g", "ln1_b", "ln2_g", "ln2_b"):
            inp(n_, [dp, c.D])
        inp("ffn_w1", [nd, c.D, c.FD]); inp("ffn_w3", [nd, c.D, c.FD]); inp("ffn_w2", [nd, c.FD, c.D])
        inp("router_w", [max(nm, 1), c.D, c.NE]); inp("router_b", [max(nm, 1), c.NE])
        inp("moe_w1", [max(nm, 1), c.NE, c.D, c.FE]); inp("moe_w3", [max(nm, 1), c.NE, c.D, c.FE])
        inp("moe_w2", [max(nm, 1), c.NE, c.FE, c.D])
        self.out = k.dram("out", [c.L, c.D], F32, kind="ExternalOutput")

        self.ident = k.sbuf("ident", [128, 128], F32)
        self.onesD = k.sbuf("onesD", [128, 128], F32)
        self.ones = k.sbuf("ones", [128, 128], F32)
        self.mkmask(self.ident, [[-1, 128]], 1, ALU.not_equal, zero_then=1.0)
        k.op("pool", lambda e: e.memset(self.onesD[:, :], 1.0 / c.D), writes=[self.onesD])
        k.op("pool", lambda e: e.memset(self.ones[:, :], 1.0), writes=[self.ones])
        self.identb = k.sbuf("identb", [128, 128], BF16)
        k.op("dve", lambda e: e.tensor_copy(out=self.identb[:, :], in_=self.ident[:, :]), reads=[self.ident], writes=[self.identb])
        self.pmark = k.mark()

    def mkmask(self, buf, pattern, chmul, cmp, zero_then=None, base=0):
        k = self.k
        if zero_then is not None:
            k.op("pool", lambda e: e.memset(buf[:, :], 0.0), writes=[buf])
            fill = zero_then
        else:
            k.op("pool", lambda e: e.memset(buf[:, :], 1.0), writes=[buf])
            fill = 0.0
        k.op("pool", lambda e: e.affine_select(out=buf[:, :], in_=buf[:, :], pattern=pattern, compare_op=cmp,
                                               fill=fill, base=base, channel_multiplier=chmul),
             reads=[buf], writes=[buf])

    def evac_eng(self):
        self.evac_rr ^= 1
        return "act" if self.evac_rr else "dve"

    def copy(self, e, out, in_):
        if e == "act":
            return lambda en: en.activation(out=out, in_=in_, func=AF.Copy)
        return lambda en: en.tensor_copy(out=out, in_=in_)

    def colvec(self, dst, dst_ap, src1d, n128, ps, tmp):
        k = self.k
        k.dma("sp", tmp[0:n128, :], src1d.rearrange("(a p) -> a p", p=128), tmp, writes=[tmp])
        k.op("pe", lambda e: e.matmul(ps[:, 0:n128], lhsT=tmp[0:n128, :], rhs=self.ident[0:n128, 0:n128],
                                      start=True, stop=True), reads=[tmp, self.ident], writes=[ps])
        k.op("dve", lambda e: e.tensor_copy(out=dst_ap, in_=ps[:, 0:n128]), reads=[ps], parts=[dst])

    def prep(self, name, w2d, K_, N_, CW=256):
        k = self.k
        kc = K_ // 128
        ng = (N_ + CW - 1) // CW
        wb = k.dram(name, [ng, 128, kc, CW], BF16)
        toks = []
        for g in range(ng):
            w = min(CW, N_ - g * CW)
            if len(toks) >= 2:
                t_ = toks[-2]
                k._emit_waits("pool", {id(t_[0]): t_})
            for k0 in range(0, kc, 32):
                k1 = min(kc, k0 + 32)
                if k0 > 0 and len(toks) >= 2:
                    t_ = toks[-2]
                    k._emit_waits("pool", {id(t_[0]): t_})
                toks.append(k.dma("pool", wb[g, :, k0:k1, 0:w],
                                  w2d[k0 * 128:k1 * 128, g * CW:g * CW + w].rearrange("(kc p) c -> p kc c", p=128),
                                  wb, parts=[wb]))
        wb.kc, wb.ng, wb.CW, wb.N = kc, ng, CW, N_
        return wb

    def stage_mods(self):
        k, c, I = self.k, self.c, self.I
        KC = c.KC
        self.modT = k.sbuf("modT", [128, c.depth * 6 * KC, 2], F32)
        self.pmark = k.mark()
        m = k.mark()
        c2s = k.sbuf("c2s", [2, c.D], F32)
        condT = k.sbuf("condT", [128, KC, 2], F32)
        adab = k.sbuf("adab", [128, c.depth * 6 * KC], F32)
        tmp = k.sbuf("cv_tmp", [128, 128], F32)
        ps = k.psum("cv_ps", [128, 512])
        psm = [k.psum("mod_ps%d" % i, [128, 512]) for i in range(2)]
        wts = [k.sbuf("adaw%d" % i, [128, KC, 128], F32) for i in range(3)]
        k.dma("sp", c2s[:, :], I["c2"][:, :], c2s, writes=[c2s])
        k.op("act", lambda e: e.activation(out=c2s[:, :], in_=c2s[:, :], func=AF.Silu), reads=[c2s], writes=[c2s])
        for kc in range(KC):
            k.op("pe", lambda e: e.matmul(ps[:, 2 * kc:2 * kc + 2], lhsT=c2s[0:2, kc * 128:(kc + 1) * 128],
                                          rhs=self.ident[0:2, 0:2], start=True, stop=True),
                 reads=[c2s, self.ident], parts=[ps])
        k.op("dve", lambda e: e.tensor_copy(out=condT[:, :, :], in_=ps[:, 0:2 * KC].rearrange("p (k r) -> p k r", r=2)),
             reads=[ps], writes=[condT])
        nj = c.depth * 6 * KC
        for a in range(0, nj, 128):
            n_ = min(128, nj - a)
            self.colvec(adab, adab[:, a:a + n_], I["ada_b"].rearrange("l n -> (l n)")[a * 128:(a + n_) * 128], n_, ps, tmp)
        for l in range(c.depth):
            for j in range(6 * KC):
                wt = wts[j % 3]
                pm = psm[j % 2]
                k.dma("sp", wt[:, :, :], I["ada_w"][l, :, j * 128:(j + 1) * 128].rearrange("(kc p) c -> p kc c", p=128),
                      wt, writes=[wt])

                def mm(e, wt=wt, pm=pm):
                    ins = None
                    for kc in range(KC):
                        ins = e.matmul(pm[:, 0:2], lhsT=wt[:, kc, :], rhs=condT[:, kc, :], start=(kc == 0), stop=(kc == KC - 1))
                    return ins
                k.op("pe", mm, reads=[wt, condT], writes=[pm])
                jj = l * 6 * KC + j
                k.op("dve", lambda e, pm=pm, jj=jj: e.tensor_scalar(out=self.modT[:, jj, :], in0=pm[:, 0:2],
                                                                  scalar1=adab[:, jj:jj + 1], scalar2=None, op0=ALU.add),
                     reads=[pm, adab], parts=[self.modT])
        k.release(m)

    def mod(self, l, which, kc, ctx):
        j = (l * 6 + which) * self.c.KC + kc
        r = 1 if ctx else 0
        return self.modT[:, j, r:r + 1]

    def stage_p0(self, RT):
        k, c, I = self.k, self.c, self.I
        KC = c.KC
        m = k.mark()
        xin = [k.sbuf("p0_in%d" % i, [128, c.D], F32) for i in range(2)]
        xo = [k.sbuf("p0_out%d" % i, [128, KC, 128], F32) for i in range(2)]
        pss = [k.psum("p0_ps%d" % i, [128, 512]) for i in range(4)]
        pi = 0
        for tt in range(c.T // 128):
            t0 = tt * 128
            xi, xoo = xin[tt % 2], xo[tt % 2]
            src = I["ctx"][t0:t0 + 128, :] if t0 < c.LC else I["x"][t0 - c.LC:t0 - c.LC + 128, :]
            k.dma("sp", xi[:, :], src, xi, writes=[xi])
            for g in range(0, KC, 4):
                ps = pss[pi % 4]; pi += 1
                nb = min(4, KC - g)

                def tr(e, ps=ps, g=g, nb=nb, xi=xi):
                    ins = None
                    for b in range(nb):
                        ins = e.transpose(ps[:, b * 128:(b + 1) * 128], xi[:, (g + b) * 128:(g + b + 1) * 128], self.ident[:, :])
                    return ins
                k.op("pe", tr, reads=[xi, self.ident], writes=[ps])
                ee = self.evac_eng()
                k.op(ee, self.copy(ee, xoo[:, g:g + nb, :], ps[:, 0:nb * 128].rearrange("p (b t) -> p b t", t=128)),
                     reads=[ps], parts=[xoo])
            k.dma("pool", RT[:, t0:t0 + 128].rearrange("(kc p) t -> p kc t", p=128), xoo[:, :, :], xoo,
                  reads=[xoo], parts=[RT])
        k.release(m)

    def dump(self, name, src, src_ap, shape, dt=F32):
        k = self.k
        o = k.dram("dbg_" + name, list(shape), dt, kind="ExternalOutput")
        k.dma("sp", o.t, src_ap, o, reads=[src], writes=[o])
        self.dbg[name] = o
        return o

    def ln_alloc(self, nsub):
        k, c = self.k, self.c
        L = types_ns()
        L.nsub = nsub
        L.rb = k.sbuf("ln_rb", [128, c.KC, nsub], F32)
        L.sq = [k.sbuf("ln_sq%d" % i, [128, 4, nsub], F32) for i in range(2)]
        L.tmp = [k.sbuf("ln_tmp%d" % i, [128, nsub], F32) for i in range(2)]
        L.mean = k.sbuf("ln_mean", [128, nsub], F32)
        L.rstd = k.sbuf("ln_rstd", [128, nsub], F32)
        L.msq = k.sbuf("ln_msq", [128, nsub], F32)
        L.ps_s = k.psum("ln_ps_s", [128, 512])
        L.ps_q = k.psum("ln_ps_q", [128, 512])
        return L

    def ln_stats(self, L, n, eps, nkc=None, src=None):
        k, c = self.k, self.c
        src = src or L.rb
        nkc = nkc or c.KC
        ones = self.onesD if nkc == c.KC else self.onesK[nkc]
        for g in range(0, nkc, 4):
            nb = min(4, nkc - g)
            sq = L.sq[(g // 4) % 2]
            k.op("act", lambda e: e.activation(out=sq[:, 0:nb, 0:n], in_=src[:, g:g + nb, 0:n], func=AF.Square),
                 reads=[src], writes=[sq])

            def mm(e, g=g, nb=nb, sq=sq):
                ins = None
                for b in range(nb):
                    kc = g + b
                    e.matmul(L.ps_s[:, 0:n], lhsT=ones[:, :], rhs=src[:, kc, 0:n], start=(kc == 0), stop=(kc == nkc - 1))
                    ins = e.matmul(L.ps_q[:, 0:n], lhsT=ones[:, :], rhs=sq[:, b, 0:n], start=(kc == 0), stop=(kc == nkc - 1))
                return ins
            if g == 0:
                k.op("pe", mm, reads=[src, sq, ones], writes=[L.ps_s, L.ps_q])
            else:
                k.op("pe", mm, reads=[src, sq, ones], parts=[L.ps_s, L.ps_q])
        k.op("dve", lambda e: e.tensor_copy(out=L.mean[:, 0:n], in_=L.ps_s[:, 0:n]), reads=[L.ps_s], writes=[L.mean])
        k.op("dve", lambda e: e.tensor_tensor(out=L.msq[:, 0:n], in0=L.mean[:, 0:n], in1=L.mean[:, 0:n], op=ALU.mult),
             reads=[L.mean], writes=[L.msq])
        k.op("dve", lambda e: e.tensor_tensor(out=L.rstd[:, 0:n], in0=L.ps_q[:, 0:n], in1=L.msq[:, 0:n], op=ALU.subtract),
             reads=[L.ps_q, L.msq], writes=[L.rstd])
        self.rsqrt(L.rstd, L.rstd[:, 0:n], eps)

    def rsqrt(self, buf, ap, eps):
        k = self.k
        k.op("dve", lambda e: e.tensor_scalar(out=ap, in0=ap, scalar1=float(eps), scalar2=None, op0=ALU.add), reads=[buf], writes=[buf])
        k.op("act", lambda e: e.activation(out=ap, in_=ap, func=AF.Sqrt), reads=[buf], writes=[buf])
        k.op("dve", lambda e: e.reciprocal(out=ap, in_=ap), reads=[buf], writes=[buf])

    def ln_apply(self, L, n, scale_fn, bias_fn, dst, dst_fn, nkc=None, src=None, func=AF.Identity):
        k, c = self.k, self.c
        src = src or L.rb
        nkc = nkc or c.KC
        for kc in range(nkc):
            tmp = L.tmp[kc % 2]
            k.op("dve", lambda e: e.tensor_tensor(out=tmp[:, 0:n], in0=src[:, kc, 0:n], in1=L.mean[:, 0:n], op=ALU.subtract),
                 reads=[src, L.mean], writes=[tmp])
            k.op("dve", lambda e: e.tensor_tensor(out=tmp[:, 0:n], in0=tmp[:, 0:n], in1=L.rstd[:, 0:n], op=ALU.mult),
                 reads=[tmp, L.rstd], writes=[tmp])
            k.op("act", lambda e: e.activation(out=dst_fn(kc), in_=tmp[:, 0:n], func=func, scale=scale_fn(kc), bias=bias_fn(kc)),
                 reads=[tmp] + self.ln_deps, parts=[dst])

    def ln_pass(self, L, src, t0, n, post, l, sub, ctx, AHT, uT, uoff, uF=None):
        k, c = self.k, self.c
        rb = L.rb
        k.dma("sp", rb[:, :, 0:n], src[:, t0:t0 + n].rearrange("(kc p) t -> p kc t", p=128), rb, reads=[src], writes=[rb])
        self.ln_deps = [self.modT, self.sc1p]
        if post is not None:
            vecs, gi, bi = post
            self.ln_deps = [self.modT, self.sc1p, vecs]
            self.ln_stats(L, n, c.EPS)
            self.ln_apply(L, n, lambda kc: vecs[:, gi, kc:kc + 1], lambda kc: vecs[:, bi, kc:kc + 1], rb,
                          lambda kc: rb[:, kc, 0:n])
        else:
            k.op("act", lambda e: e.activation(out=rb[:, :, 0:n], in_=rb[:, :, 0:n], func=AF.Copy, scale=float(c.ALPHA)),
                 reads=[rb], writes=[rb])
        if AHT is not None:
            k.dma("pool", AHT[:, t0:t0 + n].rearrange("(kc p) t -> p kc t", p=128), rb[:, :, 0:n], rb, reads=[rb], parts=[AHT])
        if uT is None:
            return
        self.ln_stats(L, n, c.EPS * c.ALPHA ** 2)
        sh, sc = (0, 1) if sub == 0 else (3, 4)
        if uF is None:
            self.ln_apply(L, n, lambda kc: self.sc1p[:, (l * 2 + sub) * c.KC + kc, (1 if ctx else 0):(1 if ctx else 0) + 1],
                          lambda kc: self.mod(l, sh, kc, ctx), uT, lambda kc: uT[:, kc, uoff:uoff + n])
        else:
            self.ln_apply(L, n, lambda kc: self.sc1p[:, (l * 2 + sub) * c.KC + kc, (1 if ctx else 0):(1 if ctx else 0) + 1],
                          lambda kc: self.mod(l, sh, kc, ctx), uF, lambda kc: uF[:, kc, 0:n])
            for k0 in range(0, c.KC, 8):
                k1 = min(c.KC, k0 + 8)
                k.op("dve", lambda e: e.tensor_copy(out=uT[:, k0:k1, uoff:uoff + n], in_=uF[:, k0:k1, 0:n]), reads=[uF], parts=[uT])

    def stage_vecs(self):
        k, c, I = self.k, self.c, self.I
        KC = c.KC
        self.vecs = [k.sbuf("vecs%d" % l, [128, 4, KC], F32) for l in range(c.depth)]
        self.vraw = [k.sbuf("vraw%d" % l, [128, 4, KC], F32) for l in range(c.depth)]
        self.sc1p = k.sbuf("sc1p", [128, c.depth * 2 * KC, 2], F32)
        self.pmark = k.mark()
        m = k.mark()
        tmp = k.sbuf("cv_tmp", [128, 128], F32)
        ps = k.psum("cv_ps", [128, 512])
        for l in range(c.depth):
            for i, nm in enumerate(("ln1_g", "ln1_b", "ln2_g", "ln2_b")):
                self.colvec(self.vraw[l], self.vraw[l][:, i, :], I[nm][l, :], KC, ps, tmp)
            k.op("act", lambda e: e.activation(out=self.vecs[l][:, :, :], in_=self.vraw[l][:, :, :], func=AF.Copy, scale=float(c.ALPHA)),
                 reads=[self.vraw[l]], writes=[self.vecs[l]])
            for sub, which in ((0, 1), (1, 4)):
                j0 = (l * 6 + which) * KC
                d0 = (l * 2 + sub) * KC
                k.op("dve", lambda e: e.tensor_scalar(out=self.sc1p[:, d0:d0 + KC, :], in0=self.modT[:, j0:j0 + KC, :],
                                                      scalar1=1.0, scalar2=None, op0=ALU.add),
                     reads=[self.modT], parts=[self.sc1p])
        k.release(m)

    def ws_tile(self, wt, ci, uT, kcn, n, pset, uoff=0):
        k = self.k
        sp = nsplit(n)

        def mm(e):
            ins = None
            for kc in range(kcn):
                for si, (o, w) in enumerate(sp):
                    ins = e.matmul(pset[si][:, 0:w], lhsT=wt[:, kc, ci * 128:(ci + 1) * 128], rhs=uT[:, kc, uoff + o:uoff + o + w],
                                   start=(kc == 0), stop=(kc == kcn - 1))
            return ins
        k.op("pe", mm, reads=[wt, uT], writes=list(pset[:len(sp)]))
        return sp

    def stage_inproj(self, l, src, post, AHT, ZDT, XBCT, GLUT):
        k, c, I = self.k, self.c, self.I
        KC = c.KC
        W = I["w_in"]
        wA = self.prep("wA%d" % l, W[l, :, 0:c.OFF_X], c.D, c.OFF_X)
        wB = self.prep("wB%d" % l, W[l, :, c.OFF_X:c.OFF_GLU], c.D, c.DXBC)
        wC = self.prep("wC%d" % l, W[l, :, c.OFF_GLU:c.OFF_GLU + c.DCF], c.D, c.DCF)
        wD = self.prep("wD%d" % l, W[l, :, c.OFF_GLU + c.DCF:c.NIN], c.D, c.DCF)
        m = k.mark()
        L = self.ln_alloc(c.NSUB)
        uT = k.sbuf("uT", [128, KC, c.TB], BF16)
        wts = [k.sbuf("wt%d" % i, [128, KC, 256], BF16) for i in range(3)]
        stg = [k.sbuf("stg%d" % i, [128, c.TB], F32) for i in range(3)]
        sgb = [k.sbuf("sg%d" % i, [128, c.TB], F32) for i in range(2)]
        stgA = [k.sbuf("stgA%d" % i, [128, (c.TB // 128) * 256], F32) for i in range(2)]
        nb = (c.TB + 511) // 512
        psets = [[k.psum("g_ps%d_%d" % (a, b), [128, 512]) for b in range(nb)] for a in range(6 // nb)]
        flat = [p for ps_ in psets for p in ps_]
        wi = 0; si = 0; pi = 0
        for bi_, (t0, n) in enumerate(tblocks(c)):
            ctx = bi_ == 0
            for o in range(0, n, c.NSUB):
                ns = min(c.NSUB, n - o)
                self.ln_pass(L, src, t0 + o, ns, post, l, 0, ctx, AHT, uT, o)
            for g in range(wB.ng):
                wt = wts[wi % 3]; wi += 1
                k.dma("sp", wt[:, :, :], wB[g], wt, reads=[wB], writes=[wt])
                for ci in range(2):
                    j = g * 2 + ci
                    if j * 128 >= c.DXBC:
                        break
                    pset = psets[pi % len(psets)]; pi += 1
                    sp = self.ws_tile(wt, ci, uT, KC, n, pset)
                    st = stg[si % 3]; si += 1
                    for s_i, (o, w) in enumerate(sp):
                        ee = self.evac_eng()
                        k.op(ee, self.copy(ee, st[:, o:o + w], pset[s_i][:, 0:w]), reads=[pset[s_i]], parts=[st])
                    k.dma("pool", XBCT[j * 128:(j + 1) * 128, t0:t0 + n], st[:, 0:n], st, reads=[st], parts=[XBCT])
            for g in range(wC.ng):
                wtc = wts[wi % 3]; wi += 1
                wtd = wts[wi % 3]; wi += 1
                k.dma("sp", wtc[:, :, :], wC[g], wtc, reads=[wC], writes=[wtc])
                k.dma("sp", wtd[:, :, :], wD[g], wtd, reads=[wD], writes=[wtd])
                for ci in range(2):
                    j = g * 2 + ci
                    if j * 128 >= c.DCF:
                        break
                    pa = psets[pi % len(psets)]; pi += 1
                    pb = psets[pi % len(psets)]; pi += 1
                    sp = self.ws_tile(wtc, ci, uT, KC, n, pa)
                    self.ws_tile(wtd, ci, uT, KC, n, pb)
                    st = stg[si % 3]; sg = sgb[si % 2]; si += 1
                    for s_i, (o, w) in enumerate(sp):
                        k.op("act", lambda e: e.activation(out=sg[:, o:o + w], in_=pb[s_i][:, 0:w], func=AF.Sigmoid),
                             reads=[pb[s_i]], parts=[sg])
                        k.op("dve", lambda e: e.tensor_tensor(out=st[:, o:o + w], in0=pa[s_i][:, 0:w], in1=sg[:, o:o + w], op=ALU.mult),
                             reads=[pa[s_i], sg], parts=[st])
                    k.dma("pool", GLUT[j * 128:(j + 1) * 128, t0:t0 + n], st[:, 0:n], st, reads=[st], parts=[GLUT])
            for g in range(wA.ng):
                wt = wts[wi % 3]; wi += 1
                k.dma("sp", wt[:, :, :], wA[g], wt, reads=[wA], writes=[wt])
                w = min(256, c.OFF_X - g * 256)
                st = stgA[si % 2]; si += 1
                ntt = n // 128
                for tt in range(ntt):
                    ps = flat[pi % len(flat)]; pi += 1

                    def mm(e, tt=tt, ps=ps, wt=wt, w=w):
                        ins = None
                        for kc in range(KC):
                            ins = e.matmul(ps[:, 0:w], lhsT=uT[:, kc, tt * 128:(tt + 1) * 128], rhs=wt[:, kc, 0:w],
                                           start=(kc == 0), stop=(kc == KC - 1))
                        return ins
                    k.op("pe", mm, reads=[wt, uT], writes=[ps])
                    ee = self.evac_eng()
                    k.op(ee, self.copy(ee, st[:, tt * w:(tt + 1) * w], ps[:, 0:w]), reads=[ps], parts=[st])
                k.dma("pool", ZDT[t0:t0 + n, g * 256:g * 256 + w].rearrange("(tt p) w -> p tt w", p=128),
                      st[:, 0:ntt * w].rearrange("p (tt w) -> p tt w", w=w), st, reads=[st], parts=[ZDT])
        k.release(m)

    def stage_ssmprep(self, l, XBCT, XTM, BTM, BT, CT):
        k, c, I = self.k, self.c, self.I
        m = k.mark()
        NT = c.DXBC // 128
        nxt = c.DS // 128
        cw = k.sbuf("cw", [128, NT, c.KS], F32)
        cb = k.sbuf("cb", [128, NT], F32)
        tmp = k.sbuf("cv_tmp", [128, 128], F32)
        ps = k.psum("cv_ps", [128, 512])
        for kk in range(c.KS):
            self.colvec(cw, cw[:, :, kk], I["conv_w"][l, kk, :], NT, ps, tmp)
        self.colvec(cb, cb[:, :], I["conv_b"][l, :], NT, ps, tmp)
        raw = [k.sbuf("sp_raw%d" % i, [128, c.T], F32) for i in range(2)]
        acc = [k.sbuf("sp_acc%d" % i, [128, c.T], F32) for i in range(2)]
        obf = [k.sbuf("sp_obf%d" % i, [128, c.T], BF16) for i in range(2)]
        tst = [k.sbuf("sp_tst%d" % i, [128, 4, 128], F32) for i in range(2)]
        tsb = [k.sbuf("sp_tsb%d" % i, [128, 4, 128], BF16) for i in range(2)]
        pst = [k.psum("sp_ps%d" % i, [128, 512]) for i in range(3)]
        segs = [(0, c.LC), (c.LC, c.T)]
        pi = 0; ti = 0
        half = c.KS // 2
        for j in range(NT):
            r, a = raw[j % 2], acc[j % 2]
            k.dma("sp", r[:, :], XBCT[j * 128:(j + 1) * 128, :], r, reads=[XBCT], writes=[r])
            k.op("dve", lambda e: e.tensor_scalar(out=a[:, :], in0=r[:, :], scalar1=cw[:, j, half:half + 1], scalar2=cb[:, j:j + 1],
                                                  op0=ALU.mult, op1=ALU.add), reads=[r, cw, cb], writes=[a])
            for kk in range(c.KS):
                d = kk - half
                if d == 0:
                    continue
                for (s0, s1) in segs:
                    lo, hi = max(s0, s0 - d), min(s1, s1 - d)
                    k.op("dve", lambda e: e.scalar_tensor_tensor(out=a[:, lo:hi], in0=r[:, lo + d:hi + d], scalar=cw[:, j, kk:kk + 1],
                                                                 in1=a[:, lo:hi], op0=ALU.mult, op1=ALU.add),
                         reads=[r, cw, a], writes=[a])
            if j < nxt:
                k.op("act", lambda e: e.activation(out=a[:, :], in_=a[:, :], func=AF.Silu), reads=[a], writes=[a])
                for t4 in range(0, c.T // 128, 4):
                    nb = min(4, c.T // 128 - t4)
                    pp = pst[pi % 3]; pi += 1
                    st = tst[ti % 2]; ti += 1

                    def tr(e, pp=pp, t4=t4, nb=nb, a=a):
                        ins = None
                        for b in range(nb):
                            ins = e.transpose(pp[:, b * 128:(b + 1) * 128], a[:, (t4 + b) * 128:(t4 + b + 1) * 128], self.ident[:, :])
                        return ins
                    k.op("pe", tr, reads=[a, self.ident], writes=[pp])
                    ee = self.evac_eng()
                    k.op(ee, self.copy(ee, st[:, 0:nb, :], pp[:, 0:nb * 128].rearrange("p (b t) -> p b t", t=128)), reads=[pp], writes=[st])
                    k.dma("pool", XTM[t4 * 128:(t4 + nb) * 128, j * 128:(j + 1) * 128].rearrange("(b p) c -> p b c", p=128),
                          st[:, 0:nb, :], st, reads=[st], parts=[XTM])
            else:
                ob = obf[j % 2]
                k.op("act", lambda e: e.activation(out=ob[:, :], in_=a[:, :], func=AF.Silu), reads=[a], writes=[ob])
                gi = j - nxt
                if gi < c.G:
                    k.dma("pool", BT[gi * 128:(gi + 1) * 128, :], ob[:, :], ob, reads=[ob], parts=[BT])
                    k.op("act", lambda e: e.activation(out=a[:, :], in_=a[:, :], func=AF.Silu), reads=[a], writes=[a])
                    for t4 in range(0, c.T // 128, 4):
                        nb = min(4, c.T // 128 - t4)
                        pp = pst[pi % 3]; pi += 1
                        st = tsb[ti % 2]; ti += 1

                        def tr(e, pp=pp, t4=t4, nb=nb, a=a):
                            ins = None
                            for b in range(nb):
                                ins = e.transpose(pp[:, b * 128:(b + 1) * 128], a[:, (t4 + b) * 128:(t4 + b + 1) * 128], self.ident[:, :])
                            return ins
                        k.op("pe", tr, reads=[a, self.ident], writes=[pp])
                        ee = self.evac_eng()
                        k.op(ee, self.copy(ee, st[:, 0:nb, :], pp[:, 0:nb * 128].rearrange("p (b t) -> p b t", t=128)), reads=[pp], writes=[st])
                        k.dma("pool", BTM[t4 * 128:(t4 + nb) * 128, gi * 128:(gi + 1) * 128].rearrange("(b p) c -> p b c", p=128),
                              st[:, 0:nb, :], st, reads=[st], parts=[BTM])
                else:
                    gi -= c.G
                    k.dma("pool", CT[gi * 128:(gi + 1) * 128, :], ob[:, :], ob, reads=[ob], parts=[CT])
        k.release(m)

    def bc_row(self, dst, src_row_ap, ncol):
        self.k.dma("sp", dst[:, 0:ncol], src_row_ap.to_broadcast([128, ncol]), dst, writes=[dst])

    def stage_ssd(self, l, ZDT, XTM, BTM, BT, CT, YB, MT):
        k, c, I = self.k, self.c, self.I
        H, G, E, DS = c.H, c.G, c.E, c.DS
        m = k.mark()
        U = k.sbuf("mU", [128, 128], F32); Lo = k.sbuf("mLo", [128, 128], F32)
        SLs = k.sbuf("mSL", [128, 128], F32); SUs = k.sbuf("mSU", [128, 128], F32)
        self.mkmask(U, [[1, 128]], -1, ALU.is_ge)
        self.mkmask(Lo, [[-1, 128]], 1, ALU.is_ge)
        self.mkmask(SLs, [[-1, 128]], 1, ALU.is_gt)
        self.mkmask(SUs, [[1, 128]], -1, ALU.is_gt)
        dtb = k.sbuf("dtb", [128, 2 * H], F32); negA = k.sbuf("negA", [128, 2 * H], F32)
        dsk = k.sbuf("dsk", [128, H], F32); nw = k.sbuf("nw", [128, DS], F32)
        self.bc_row(dtb, I["dtb_f"][l:l + 1, :], H)
        k.dma("sp", dtb[:, H:2 * H], I["dtb_b"][l:l + 1, :].to_broadcast([128, H]), dtb, parts=[dtb])
        self.bc_row(negA, I["alog_f"][l:l + 1, :], H)
        k.dma("sp", negA[:, H:2 * H], I["alog_b"][l:l + 1, :].to_broadcast([128, H]), negA, parts=[negA])
        k.op("act", lambda e: e.activation(out=negA[:, :], in_=negA[:, :], func=AF.Exp), reads=[negA], writes=[negA])
        k.op("dve", lambda e: e.tensor_scalar(out=negA[:, :], in0=negA[:, :], scalar1=-1.0, scalar2=None, op0=ALU.mult),
             reads=[negA], writes=[negA])
        self.bc_row(dsk, I["dskip"][l:l + 1, :], H)
        self.bc_row(nw, I["ssm_norm_w"][l:l + 1, :], DS)
        Hst = k.sbuf("Hst", [128, DS], F32); Hbf = k.sbuf("Hbf", [128, DS], BF16)
        Xs = [k.sbuf("s_x%d" % i, [128, DS], F32) for i in range(2)]
        Bm = [k.sbuf("s_bm%d" % i, [128, G * 128], BF16) for i in range(2)]
        Bt = [k.sbuf("s_bt%d" % i, [128, G, 128], BF16) for i in range(2)]
        Ct = [k.sbuf("s_ct%d" % i, [128, G, 128], BF16) for i in range(2)]
        Zd = [k.sbuf("s_zd%d" % i, [128, c.OFF_X], F32) for i in range(2)]
        Yb = [k.sbuf("s_yb%d" % i, [128, DS], F32) for i in range(2)]
        sm = k.sbuf("s_sm", [128, 8, H], F32)
        xdt = k.sbuf("s_xdt", [128, DS], BF16); xw = k.sbuf("s_xw", [128, DS], BF16)
        aU = [k.sbuf("s_aU%d" % i, [128, E, 128], F32) for i in range(2)]
        Ed = [k.sbuf("s_Ed%d" % i, [128, E, 128], F32) for i in range(2)]
        cbm = [k.sbuf("s_cbm%d" % i, [128, 128], F32) for i in range(2)]
        MTl = [k.sbuf("s_MT%d" % i, [128, E, 128], BF16) for i in range(2)]
        yt = k.sbuf("s_y", [128, DS], F32); ytmp = [k.sbuf("s_ytmp%d" % i, [128, E * 64], F32) for i in range(2)]
        htmp = [k.sbuf("s_htmp%d" % i, [128, E * 64], F32) for i in range(2)]
        gsq = k.sbuf("s_gsq", [128, DS], F32); ssq = k.sbuf("s_ssq", [128, G], F32)
        gbf = k.sbuf("s_gbf", [128, DS], BF16); mst = [k.sbuf("s_mst%d" % i, [128, DS // 128, 128], BF16) for i in range(2)]
        ps_sm = k.psum("s_ps_sm", [128, 512])
        ps_D = [k.psum("s_psD%d" % i, [128, 512]) for i in range(2)]
        ps_cb1 = k.psum("s_pscb", [128, 512])
        ps_Y = [k.psum("s_psY%d" % i, [128, 512]) for i in range(2)]
        ps_S = k.psum("s_psS", [128, 512])
        ps_T = k.psum("s_psT", [128, 512], BF16)
        nch = c.T // 128
        ncc = c.LC // 128
        order_b = list(range(ncc - 1, -1, -1)) + list(range(nch - 1, ncc - 1, -1))
        order_f = list(range(nch))
        gi_ = 0
        for d, order in ((1, order_b), (0, order_f)):
            k.op("pool", lambda e: e.memset(Hst[:, :], 0.0), writes=[Hst])
            k.op("pool", lambda e: e.memset(Hbf[:, :], 0.0), writes=[Hbf])
            mA, mS = (U, SLs) if d == 0 else (Lo, SUs)
            mK = U if d == 0 else Lo
            for ci, ch in enumerate(order):
                t0 = ch * 128
                b2 = ci % 2
                X, BM, BTt, CTt, ZD = Xs[b2], Bm[b2], Bt[b2], Ct[b2], Zd[b2]
                k.dma("sp", X[:, :], XTM[t0:t0 + 128, :], X, reads=[XTM], writes=[X])
                k.dma("sp", BM[:, :], BTM[t0:t0 + 128, :], BM, reads=[BTM], writes=[BM])
                k.dma("sp", BTt[:, :, :], BT[:, t0:t0 + 128].rearrange("(g n) t -> n g t", n=128), BTt, reads=[BT], writes=[BTt])
                k.dma("sp", CTt[:, :, :], CT[:, t0:t0 + 128].rearrange("(g n) t -> n g t", n=128), CTt, reads=[CT], writes=[CTt])
                k.dma("sp", ZD[:, :], ZDT[t0:t0 + 128, :], ZD, reads=[ZDT], writes=[ZD])
                if d == 0:
                    YBt = Yb[b2]
                    k.dma("sp", YBt[:, :], YB[t0:t0 + 128, :], YBt, reads=[YB], writes=[YBt])
                k.op("dve", lambda e: e.tensor_tensor(out=sm[:, 0, :], in0=ZD[:, c.OFF_DT + d * H:c.OFF_DT + (d + 1) * H],
                                                      in1=dtb[:, d * H:(d + 1) * H], op=ALU.add), reads=[ZD, dtb], writes=[sm])
                k.op("act", lambda e: e.activation(out=sm[:, 0, :], in_=sm[:, 0, :], func=AF.Exp), reads=[sm], writes=[sm])
                k.op("act", lambda e: e.activation(out=sm[:, 0, :], in_=sm[:, 0, :], func=AF.Ln, bias=1.0), reads=[sm], writes=[sm])
                k.op("dve", lambda e: e.tensor_tensor(out=sm[:, 1, :], in0=sm[:, 0, :], in1=negA[:, d * H:(d + 1) * H], op=ALU.mult),
                     reads=[sm, negA], writes=[sm])
                def mm3(e):
                    e.matmul(ps_sm[:, 0:H], lhsT=mA[:, :], rhs=sm[:, 1, :], start=True, stop=True)
                    return e.matmul(ps_sm[:, H:2 * H], lhsT=self.ones[:, :], rhs=sm[:, 1, :], start=True, stop=True)
                k.op("pe", mm3, reads=[mA, self.ones, sm], writes=[ps_sm])
                k.op("dve", lambda e: e.tensor_copy(out=sm[:, 2, :], in_=ps_sm[:, 0:H]), reads=[ps_sm], writes=[sm])
                k.op("dve", lambda e: e.tensor_copy(out=sm[:, 4, :], in_=ps_sm[:, H:2 * H]), reads=[ps_sm], writes=[sm])
                k.op("dve", lambda e: e.tensor_tensor(out=sm[:, 3, :], in0=sm[:, 4, :], in1=sm[:, 2, :], op=ALU.subtract),
                     reads=[sm], writes=[sm])
                k.op("act", lambda e: e.activation(out=sm[:, 5:8, :], in_=sm[:, 2:5, :], func=AF.Exp), reads=[sm], writes=[sm])
                k.op("dve", lambda e: e.tensor_tensor(out=sm[:, 3, :], in0=sm[:, 0, :], in1=sm[:, 6, :], op=ALU.mult), reads=[sm], writes=[sm])
                X3 = X[:, :].rearrange("p (h q) -> p h q", q=64)
                k.op("dve", lambda e: e.tensor_tensor(out=xdt[:, :].rearrange("p (h q) -> p h q", q=64), in0=X3,
                                                      in1=sm[:, 0, :].rearrange("p (h o) -> p h o", o=1).to_broadcast([128, H, 64]), op=ALU.mult),
                     reads=[X, sm], writes=[xdt])
                k.op("pool", lambda e: e.tensor_tensor(out=xw[:, :].rearrange("p (h q) -> p h q", q=64), in0=X3,
                                                       in1=sm[:, 3, :].rearrange("p (h o) -> p h o", o=1).to_broadcast([128, H, 64]), op=ALU.mult),
                     reads=[X, sm], writes=[xw])
                for g in range(G):
                    g2 = gi_ % 2; gi_ += 1
                    hs = slice(g * E, (g + 1) * E)
                    cs = slice(g * E * 64, (g + 1) * E * 64)
                    k.op("pool", lambda e: e.tensor_tensor(out=aU[g2][:, :, :],
                                                           in0=mA[:, :].rearrange("p (o j) -> p o j", o=1).to_broadcast([128, E, 128]),
                                                           in1=sm[:, 1, hs].rearrange("p (h o) -> p h o", o=1).to_broadcast([128, E, 128]),
                                                           op=ALU.mult), reads=[mA, sm], writes=[aU[g2]])
                    k.op("pe", lambda e: e.matmul(ps_D[g2][:, 0:E * 128], lhsT=mS[:, :], rhs=aU[g2][:, :, :].rearrange("p h j -> p (h j)"),
                                                  start=True, stop=True), reads=[mS, aU[g2]], writes=[ps_D[g2]])
                    k.op("act", lambda e: e.activation(out=Ed[g2][:, :, :].rearrange("p h j -> p (h j)"), in_=ps_D[g2][:, 0:E * 128], func=AF.Exp),
                         reads=[ps_D[g2]], writes=[Ed[g2]])
                    cq = (gi_ % 4) * 128
                    k.op("pe", lambda e: e.matmul(ps_cb1[:, cq:cq + 128], lhsT=BTt[:, g, :], rhs=CTt[:, g, :], start=True, stop=True),
                         reads=[BTt, CTt], parts=[ps_cb1])
                    k.op("dve", lambda e: e.tensor_tensor(out=cbm[g2][:, :], in0=ps_cb1[:, cq:cq + 128], in1=mK[:, :], op=ALU.mult),
                         reads=[ps_cb1, mK], writes=[cbm[g2]])
                    k.op("dve", lambda e: e.tensor_tensor(out=MTl[g2][:, :, :], in0=Ed[g2][:, :, :],
                                                          in1=cbm[g2][:, :].rearrange("p (o j) -> p o j", o=1).to_broadcast([128, E, 128]),
                                                          op=ALU.mult), reads=[Ed[g2], cbm[g2]], writes=[MTl[g2]])
                    def mmY(e, g=g, g2=g2):
                        for e_ in range(E):
                            e.matmul(ps_Y[g2][:, e_ * 64:(e_ + 1) * 64], lhsT=MTl[g2][:, e_, :],
                                     rhs=xdt[:, (g * E + e_) * 64:(g * E + e_ + 1) * 64], start=True, stop=True)
                        return e.matmul(ps_Y[g2][:, 256:256 + E * 64], lhsT=CTt[:, g, :], rhs=Hbf[:, g * E * 64:(g + 1) * E * 64],
                                        start=True, stop=True)
                    k.op("pe", mmY, reads=[MTl[g2], xdt, CTt, Hbf], writes=[ps_Y[g2]])
                    k.op("dve", lambda e: e.tensor_tensor(out=ytmp[g2][:, :].rearrange("p (h q) -> p h q", q=64),
                                                          in0=ps_Y[g2][:, 256:256 + E * 64].rearrange("p (h q) -> p h q", q=64),
                                                          in1=sm[:, 5, hs].rearrange("p (h o) -> p h o", o=1).to_broadcast([128, E, 64]),
                                                          op=ALU.mult), reads=[ps_Y[g2], sm], writes=[ytmp[g2]])
                    k.op("dve", lambda e: e.tensor_tensor(out=yt[:, cs], in0=ytmp[g2][:, :], in1=ps_Y[g2][:, 0:E * 64], op=ALU.add),
                         reads=[ytmp[g2], ps_Y[g2]], parts=[yt])
                for g in range(G):
                    cs = slice(g * E * 64, (g + 1) * E * 64)
                    hs = slice(g * E, (g + 1) * E)
                    g2 = g % 2
                    k.op("pe", lambda e: e.matmul(ps_S[:, g2 * 256:g2 * 256 + E * 64], lhsT=BM[:, g * 128:(g + 1) * 128], rhs=xw[:, cs], start=True, stop=True),
                         reads=[BM, xw], parts=[ps_S])
                    k.op("pool", lambda e: e.tensor_tensor(out=htmp[g2][:, :].rearrange("p (h q) -> p h q", q=64),
                                                           in0=Hst[:, cs].rearrange("p (h q) -> p h q", q=64),
                                                           in1=sm[:, 7, hs].rearrange("p (h o) -> p h o", o=1).to_broadcast([128, E, 64]),
                                                           op=ALU.mult), reads=[Hst, sm], writes=[htmp[g2]])
                    k.op("dve", lambda e: e.tensor_tensor(out=Hst[:, cs], in0=htmp[g2][:, :], in1=ps_S[:, g2 * 256:g2 * 256 + E * 64], op=ALU.add),
                         reads=[htmp[g2], ps_S], writes=[Hst])
                k.op("act", lambda e: e.activation(out=Hbf[:, :], in_=Hst[:, :], func=AF.Copy), reads=[Hst], writes=[Hbf])
                if d == 1:
                    k.dma("pool", YB[t0:t0 + 128, :], yt[:, :], yt, reads=[yt], parts=[YB])
                else:
                    k.op("dve", lambda e: e.tensor_tensor(out=yt[:, :], in0=yt[:, :], in1=YBt[:, :], op=ALU.add), reads=[yt, YBt], writes=[yt])
                    k.op("pool", lambda e: e.tensor_tensor(out=gsq[:, :].rearrange("p (h q) -> p h q", q=64), in0=X3,
                                                           in1=dsk[:, :].rearrange("p (h o) -> p h o", o=1).to_broadcast([128, H, 64]), op=ALU.mult),
                         reads=[X, dsk], writes=[gsq])
                    k.op("dve", lambda e: e.tensor_tensor(out=yt[:, :], in0=yt[:, :], in1=gsq[:, :], op=ALU.add), reads=[yt, gsq], writes=[yt])
                    k.op("act", lambda e: e.activation(out=gsq[:, :], in_=ZD[:, 0:DS], func=AF.Silu), reads=[ZD], writes=[gsq])
                    k.op("dve", lambda e: e.tensor_tensor(out=yt[:, :], in0=yt[:, :], in1=gsq[:, :], op=ALU.mult), reads=[yt, gsq], writes=[yt])
                    k.op("act", lambda e: e.activation(out=gsq[:, :], in_=yt[:, :], func=AF.Square), reads=[yt], writes=[gsq])
                    k.op("dve", lambda e: e.tensor_reduce(out=ssq[:, :], in_=gsq[:, :].rearrange("p (g q) -> p g q", g=G), axis=AX.X, op=ALU.add),
                         reads=[gsq], writes=[ssq])
                    k.op("dve", lambda e: e.tensor_scalar(out=ssq[:, :], in0=ssq[:, :], scalar1=1.0 / (DS // G), scalar2=None, op0=ALU.mult),
                         reads=[ssq], writes=[ssq])
                    self.rsqrt(ssq, ssq[:, :], c.EPS)
                    k.op("dve", lambda e: e.tensor_tensor(out=yt[:, :].rearrange("p (g q) -> p g q", g=G), in0=yt[:, :].rearrange("p (g q) -> p g q", g=G),
                                                          in1=ssq[:, :].rearrange("p (g o) -> p g o", o=1).to_broadcast([128, G, DS // G]), op=ALU.mult),
                         reads=[yt, ssq], writes=[yt])
                    k.op("pool", lambda e: e.tensor_tensor(out=gbf[:, :], in0=yt[:, :], in1=nw[:, :], op=ALU.mult), reads=[yt, nw], writes=[gbf])
                    ms = mst[ci % 2]
                    for j4 in range(0, DS // 128, 4):
                        nb = min(4, DS // 128 - j4)

                        def tr(e, j4=j4, nb=nb):
                            ins = None
                            for b in range(nb):
                                ins = e.transpose(ps_T[:, b * 128:(b + 1) * 128], gbf[:, (j4 + b) * 128:(j4 + b + 1) * 128], self.identb[:, :])
                            return ins
                        k.op("pe", tr, reads=[gbf, self.identb], writes=[ps_T])
                        k.op("act", lambda e: e.activation(out=ms[:, j4:j4 + nb, :], in_=ps_T[:, 0:nb * 128].rearrange("p (b t) -> p b t", t=128), func=AF.Copy),
                             reads=[ps_T], parts=[ms])
                    k.dma("pool", MT[0:DS, t0:t0 + 128].rearrange("(j p) t -> p j t", p=128), ms[:, :, :], ms, reads=[ms], parts=[MT])
        k.release(m)

    def stage_conf(self, l, GLUT, VT, MT, skip_ctx=False):
        k, c, I = self.k, self.c, self.I
        m = k.mark()
        NT = c.DCF // 128
        KW, half = c.KW, c.KW // 2
        ccw = k.sbuf("ccw", [128, NT, KW], F32)
        cvec = k.sbuf("cvec", [128, 3, NT], F32)
        tmp = k.sbuf("cv_tmp", [128, 128], F32)
        ps = k.psum("cv_ps", [128, 512])
        for kk in range(KW):
            self.colvec(ccw, ccw[:, :, kk], I["cconv_w"][l, kk, :], NT, ps, tmp)
        for i, nm in enumerate(("cconv_b", "cln_g", "cln_b")):
            self.colvec(cvec, cvec[:, i, :], I[nm][l, :], NT, ps, tmp)
        if not hasattr(self, "onesK"):
            self.onesK = {}
        m2 = k.mark()
        raw = [k.sbuf("cf_raw%d" % i, [128, c.T], F32) for i in range(2)]
        accA = [k.sbuf("cf_accA%d" % i, [128, c.T], F32) for i in range(2)]
        vertical = (l % 2 == 1)
        LC, L, GW = c.LC, c.L, c.GW
        for j in range(NT):
            r, a = raw[j % 2], accA[j % 2]
            k.dma("sp", r[:, :], GLUT[j * 128:(j + 1) * 128, :], r, reads=[GLUT], writes=[r])
            k.op("dve", lambda e: e.tensor_scalar(out=a[:, :], in0=r[:, :], scalar1=ccw[:, j, half:half + 1], scalar2=cvec[:, 0, j:j + 1],
                                                  op0=ALU.mult, op1=ALU.add), reads=[r, ccw, cvec], writes=[a])
            ti = 0
            for kk in range(KW):
                d = kk - half
                if d == 0:
                    continue
                ti += 1
                eng, acc = ("dve", a)
                ranges = []
                lo, hi = max(0, -d), min(LC, LC - d)
                if hi > lo:
                    ranges.append((acc[:, lo:hi], r[:, lo + d:hi + d], acc[:, lo:hi]))
                if vertical:
                    sh = d * GW
                    lo, hi = max(0, -sh), min(L, L - sh)
                    if hi > lo:
                        ranges.append((acc[:, LC + lo:LC + hi], r[:, LC + lo + sh:LC + hi + sh], acc[:, LC + lo:LC + hi]))
                else:
                    lo, hi = max(0, -d), min(GW, GW - d)
                    av = acc[:, LC:].rearrange("p (r w) -> p r w", w=GW)
                    rv = r[:, LC:].rearrange("p (r w) -> p r w", w=GW)
                    ranges.append((av[:, :, lo:hi], rv[:, :, lo + d:hi + d], av[:, :, lo:hi]))
                for (o_, i_, a_) in ranges:
                    k.op(eng, lambda e: e.scalar_tensor_tensor(out=o_, in0=i_, scalar=ccw[:, j, kk:kk + 1], in1=a_, op0=ALU.mult, op1=ALU.add),
                         reads=[r, ccw, acc], writes=[acc])
            k.dma("pool", VT[j * 128:(j + 1) * 128, :], a[:, :], a, reads=[a], parts=[VT])
        k.release(m2)
        nsub = 512
        L_ = self.ln_alloc(nsub)
        ok = k.sbuf("onesK%d" % NT, [128, 128], F32)
        k.op("pool", lambda e: e.memset(ok[:, :], 1.0 / (NT * 128)), writes=[ok])
        self.onesK[NT] = ok
        vb = [k.sbuf("cf_vb%d" % i, [128, NT, nsub], F32) for i in range(2)]
        ob = [k.sbuf("cf_ob%d" % i, [128, NT, nsub], BF16) for i in range(2)]
        self.ln_deps = [cvec]
        bi = 0
        for (t0, n) in tblocks(c, nsub):
            if skip_ctx and t0 < c.LC:
                continue
            v, o = vb[bi % 2], ob[bi % 2]; bi += 1
            k.dma("sp", v[:, :, 0:n], VT[:, t0:t0 + n].rearrange("(j p) t -> p j t", p=128), v, reads=[VT], writes=[v])
            self.ln_stats(L_, n, c.EPS, nkc=NT, src=v)
            self.ln_apply(L_, n, lambda kc: cvec[:, 1, kc:kc + 1], lambda kc: cvec[:, 2, kc:kc + 1], o, lambda kc: o[:, kc, 0:n],
                          nkc=NT, src=v, func=AF.Silu)
            k.dma("pool", MT[c.DS:c.D, t0:t0 + n].rearrange("(j p) t -> p j t", p=128), o[:, :, 0:n], o, reads=[o], parts=[MT])
        k.release(m)

    def stage_proj_res(self, name, l, wsrc, Kdim, MTin, gate_which, AHT, RTout, tb, skip_ctx=False):
        k, c = self.k, self.c
        KC = c.KC
        kcn = Kdim // 128
        wb = self.prep(name, wsrc, Kdim, c.D, CW=128)
        m = k.mark()
        mT = [k.sbuf("pr_mT%d" % i, [128, kcn, tb], BF16) for i in range(1)]
        wts = [k.sbuf("pr_wt%d" % i, [128, kcn, 128], BF16) for i in range(3)]
        aht = [k.sbuf("pr_ah%d" % i, [128, tb], F32) for i in range(3)]
        stg = [k.sbuf("pr_st%d" % i, [128, tb], F32) for i in range(3)]
        nb = (tb + 511) // 512
        psets = [[k.psum("pr_ps%d_%d" % (a, b), [128, 512]) for b in range(nb)] for a in range(max(2, 8 // nb // 2))]
        it = 0
        for bi_, (t0, n) in enumerate(tblocks(c, tb)):
            ctx = t0 < c.LC
            if ctx and skip_ctx:
                continue
            mt = mT[0]
            for k0 in range(0, kcn, 32):
                k1 = min(kcn, k0 + 32)
                k.dma("sp", mt[:, k0:k1, 0:n], MTin[k0 * 128:k1 * 128, t0:t0 + n].rearrange("(kc p) t -> p kc t", p=128), mt,
                      reads=[MTin], **({"writes": [mt]} if k0 == 0 else {"parts": [mt]}))
            for j in range(KC):
                wt, ah, st = wts[it % 3], aht[it % 3], stg[it % 3]
                pset = psets[it % len(psets)]; it += 1
                k.dma("sp", wt[:, :, :], wb[j], wt, reads=[wb], writes=[wt])
                k.dma("sp", ah[:, 0:n], AHT[j * 128:(j + 1) * 128, t0:t0 + n], ah, reads=[AHT], writes=[ah])
                sp = self.ws_tile(wt, 0, mt, kcn, n, pset)
                for s_i, (o, w) in enumerate(sp):
                    k.op("dve", lambda e: e.scalar_tensor_tensor(out=st[:, o:o + w], in0=pset[s_i][:, 0:w], scalar=self.mod(l, gate_which, j, ctx),
                                                                 in1=ah[:, o:o + w], op0=ALU.mult, op1=ALU.add),
                         reads=[pset[s_i], ah, self.modT], parts=[st])
                k.dma("pool", RTout[j * 128:(j + 1) * 128, t0:t0 + n], st[:, 0:n], st, reads=[st], parts=[RTout])
        k.release(m)

    def stage_ffn_up(self, l, src, AHT2, GT, experts, skip_ctx=False, router=None, nsub=None, tb=None):
        k, c, I = self.k, self.c, self.I
        KC = c.KC
        preps = []
        for ei, (w1, w3, Fe) in enumerate(experts):
            preps.append((self.prep("w1b%d_%d" % (l, ei), w1, c.D, Fe), self.prep("w3b%d_%d" % (l, ei), w3, c.D, Fe), Fe))
        m = k.mark()
        nsub = nsub or c.NSUB
        TB_ = tb or c.TB
        L = self.ln_alloc(nsub)
        uT = k.sbuf("uT", [128, KC, TB_], BF16)
        NW = 3 if router is not None else 4
        wts = [k.sbuf("wt%d" % i, [128, KC, 256], BF16) for i in range(NW)]
        sgb = [k.sbuf("sg%d" % i, [128, TB_], F32) for i in range(2)]
        stg = [k.sbuf("stgb%d" % i, [128, TB_], BF16) for i in range(3)]
        nb = (TB_ + 511) // 512
        psets = [[k.psum("g_ps%d_%d" % (a, b), [128, 512]) for b in range(nb)] for a in range(6 // nb)]
        NE = len(experts)
        if router is not None:
            rw = k.sbuf("rw", [128, KC, NE], F32)
            rwr = k.sbuf("rwr", [KC, 128 * NE], F32)
            k.dma("sp", rwr[:, :], router[0].rearrange("(kc p) e -> kc (p e)", p=128), rwr, writes=[rwr])
            for e_ in range(NE):
                k.op("pe", lambda e: e.matmul(L.ps_s[:, 0:KC], lhsT=rwr[0:KC, :].rearrange("k (p e) -> k p e", e=NE)[:, :, e_],
                                              rhs=self.ident[0:KC, 0:KC], start=True, stop=True),
                     reads=[rwr, self.ident], writes=[L.ps_s])
                k.op("dve", lambda e: e.tensor_copy(out=rw[:, :, e_], in_=L.ps_s[:, 0:KC]), reads=[L.ps_s], parts=[rw])
            rb = k.sbuf("rb", [128, NE], F32)
            self.bc_row(rb, router[1], NE)
            uF = k.sbuf("uF", [128, KC, nsub], F32)
            gts = k.sbuf("gts", [128, TB_ // 128, NE], F32)
            gsm = k.sbuf("gsm", [128, 8, NE], F32)
            gm = k.sbuf("gm", [128, 4], F32)
            gb = [k.sbuf("gb%d" % i, [128, TB_], F32) for i in range(2)]
        post = (self.vecs[l], 0, 1)
        wi = 0; si = 0; pi = 0
        for bi_, (t0, n) in enumerate(tblocks(c, TB_)):
            ctx = t0 < c.LC
            if ctx and skip_ctx:
                continue
            for o in range(0, n, nsub):
                ns = min(nsub, n - o)
                self.ln_pass(L, src, t0 + o, ns, post, l, 1, ctx, AHT2, uT, o, uF=(uF if router is not None else None))
                if router is not None:
                    for tt in range(ns // 128):
                        ps = L.ps_s
                        def mmr(e, tt=tt):
                            ins = None
                            for kc in range(KC):
                                ins = e.matmul(ps[:, 0:NE], lhsT=uF[:, kc, tt * 128:(tt + 1) * 128], rhs=rw[:, kc, :], start=(kc == 0), stop=(kc == KC - 1))
                            return ins
                        k.op("pe", mmr, reads=[uF, rw], writes=[ps])
                        self.top2(ps, rb, gsm, gm, gts, (o // 128) + tt, NE)
            for ei, (w1b, w3b, Fe) in enumerate(preps):
                if router is not None:
                    g_ = gb[ei % 2]
                    pset = psets[pi % len(psets)]; pi += 1
                    def mmg(e, pset=pset):
                        ins = None
                        for tt in range(n // 128):
                            ins = e.matmul(pset[tt // 4][:, (tt % 4) * 128:(tt % 4 + 1) * 128],
                                           lhsT=gts[:, tt, ei:ei + 1].to_broadcast([128, 128]), rhs=self.ident[:, :], start=True, stop=True)
                        return ins
                    k.op("pe", mmg, reads=[gts, self.ident], writes=list(pset))
                    for s_i, (o, w) in enumerate(nsplit(n)):
                        k.op("dve", lambda e: e.tensor_copy(out=g_[:, o:o + w], in_=pset[s_i][:, 0:w]), reads=[pset[s_i]], parts=[g_])
                row0 = sum(p_[2] for p_ in preps[:ei])
                for g in range(w1b.ng):
                    wtc = wts[wi % NW]; wi += 1
                    wtd = wts[wi % NW]; wi += 1
                    k.dma("sp", wtc[:, :, :], w1b[g], wtc, reads=[w1b], writes=[wtc])
                    k.dma("sp", wtd[:, :, :], w3b[g], wtd, reads=[w3b], writes=[wtd])
                    for ci in range(2):
                        j = g * 2 + ci
                        if j * 128 >= Fe:
                            break
                        pa = psets[pi % len(psets)]; pi += 1
                        pb = psets[pi % len(psets)]; pi += 1
                        sp = self.ws_tile(wtc, ci, uT, KC, n, pa)
                        self.ws_tile(wtd, ci, uT, KC, n, pb)
                        st = stg[si % 3]; sg = sgb[si % 2]; si += 1
                        for s_i, (o, w) in enumerate(sp):
                            k.op("act", lambda e: e.activation(out=sg[:, o:o + w], in_=pa[s_i][:, 0:w], func=AF.Silu),
                                 reads=[pa[s_i]], parts=[sg])
                            if router is None:
                                k.op("dve", lambda e: e.tensor_tensor(out=st[:, o:o + w], in0=pb[s_i][:, 0:w], in1=sg[:, o:o + w], op=ALU.mult),
                                     reads=[pb[s_i], sg], parts=[st])
                            else:
                                k.op("dve", lambda e: e.tensor_tensor(out=sg[:, o:o + w], in0=pb[s_i][:, 0:w], in1=sg[:, o:o + w], op=ALU.mult),
                                     reads=[pb[s_i], sg], parts=[sg])
                                k.op("pool", lambda e: e.tensor_tensor(out=st[:, o:o + w], in0=sg[:, o:o + w], in1=g_[:, o:o + w], op=ALU.mult),
                                     reads=[sg, g_], parts=[st])
                        k.dma("pool", GT[ei][j * 128:(j + 1) * 128, t0:t0 + n], st[:, 0:n], st, reads=[st], parts=[GT[ei]])
        k.release(m)

    def top2(self, ps, rb, gsm, gm, gts, slot, NE):
        k = self.k
        lg, m1k, l2, m2k, tmp = gsm[:, 0, :], gsm[:, 1, :], gsm[:, 2, :], gsm[:, 3, :], gsm[:, 4, :]
        k.op("dve", lambda e: e.tensor_tensor(out=lg, in0=ps[:, 0:NE], in1=rb[:, 0:NE], op=ALU.add), reads=[ps, rb], writes=[gsm])
        k.op("dve", lambda e: e.tensor_reduce(out=gm[:, 0:1], in_=lg, axis=AX.X, op=ALU.max), reads=[gsm], writes=[gm])
        k.op("dve", lambda e: e.tensor_scalar(out=m1k, in0=lg, scalar1=gm[:, 0:1], scalar2=None, op0=ALU.is_equal), reads=[gsm, gm], writes=[gsm])
        k.op("dve", lambda e: e.scalar_tensor_tensor(out=l2, in0=m1k, scalar=-1e30, in1=lg, op0=ALU.mult, op1=ALU.add), reads=[gsm], writes=[gsm])
        k.op("dve", lambda e: e.tensor_reduce(out=gm[:, 1:2], in_=l2, axis=AX.X, op=ALU.max), reads=[gsm], writes=[gm])
        k.op("dve", lambda e: e.tensor_scalar(out=m2k, in0=l2, scalar1=gm[:, 1:2], scalar2=None, op0=ALU.is_equal), reads=[gsm, gm], writes=[gsm])
        k.op("dve", lambda e: e.tensor_tensor(out=gm[:, 2:3], in0=gm[:, 1:2], in1=gm[:, 0:1], op=ALU.subtract), reads=[gm], writes=[gm])
        k.op("act", lambda e: e.activation(out=gm[:, 2:3], in_=gm[:, 2:3], func=AF.Sigmoid), reads=[gm], writes=[gm])
        k.op("dve", lambda e: e.tensor_scalar(out=gm[:, 3:4], in0=gm[:, 2:3], scalar1=-1.0, scalar2=1.0, op0=ALU.mult, op1=ALU.add), reads=[gm], writes=[gm])
        k.op("dve", lambda e: e.tensor_scalar(out=tmp, in0=m2k, scalar1=gm[:, 2:3], scalar2=None, op0=ALU.mult), reads=[gsm, gm], writes=[gsm])
        k.op("dve", lambda e: e.scalar_tensor_tensor(out=gts[:, slot, :], in0=m1k, scalar=gm[:, 3:4], in1=tmp, op0=ALU.mult, op1=ALU.add),
             reads=[gsm, gm], parts=[gts])

    def stage_final(self, l, src):
        k, c = self.k, self.c
        KC = c.KC
        m = k.mark()
        nsub = c.NSUB
        L = self.ln_alloc(nsub)
        hb = k.sbuf("fin_h", [128, KC, nsub], F32)
        ot = [k.sbuf("fin_o%d" % i, [128, c.D], F32) for i in range(2)]
        pss = [k.psum("fin_ps%d" % i, [128, 512]) for i in range(4)]
        vr = self.vraw[l]
        self.ln_deps = [vr]
        pi = 0; oi = 0
        for (t0, n) in tblocks(c, nsub):
            if t0 < c.LC:
                continue
            k.dma("sp", L.rb[:, :, 0:n], src[:, t0:t0 + n].rearrange("(kc p) t -> p kc t", p=128), L.rb, reads=[src], writes=[L.rb])
            self.ln_stats(L, n, c.EPS)
            self.ln_apply(L, n, lambda kc: vr[:, 2, kc:kc + 1], lambda kc: vr[:, 3, kc:kc + 1], hb, lambda kc: hb[:, kc, 0:n])
            for tt in range(n // 128):
                o = ot[oi % 2]; oi += 1
                for g in range(0, KC, 4):
                    nb = min(4, KC - g)
                    ps = pss[pi % 4]; pi += 1

                    def tr(e, ps=ps, g=g, nb=nb, tt=tt):
                        ins = None
                        for b in range(nb):
                            ins = e.transpose(ps[:, b * 128:(b + 1) * 128], hb[:, g + b, tt * 128:(tt + 1) * 128], self.ident[:, :])
                        return ins
                    k.op("pe", tr, reads=[hb, self.ident], writes=[ps])
                    ee = self.evac_eng()
                    k.op(ee, self.copy(ee, o[:, g * 128:(g + nb) * 128], ps[:, 0:nb * 128]), reads=[ps], parts=[o])
                k.dma("pool", self.out[t0 - c.LC + tt * 128:t0 - c.LC + (tt + 1) * 128, :], o[:, :], o, reads=[o], parts=[self.out])
        k.release(m)

    def stage_moe_down(self, l, GT, w2list, Fe, AHT, RTout, skip_ctx=False, tb=512):
        k, c = self.k, self.c
        KC = c.KC
        kcn = Fe // 128
        wbs = [self.prep("w2m%d_%d" % (l, ei), w2, Fe, c.D, CW=128) for ei, w2 in enumerate(w2list)]
        m = k.mark()
        acc = k.sbuf("md_acc", [128, KC, tb], F32)
        gT = [k.sbuf("md_gT%d" % i, [128, kcn, tb], BF16) for i in range(2)]
        wts = [k.sbuf("md_wt%d" % i, [128, kcn, 128], BF16) for i in range(4)]
        pss = [k.psum("md_ps%d" % i, [128, 512]) for i in range(4)]
        it = 0; gi = 0
        for (t0, n) in tblocks(c, tb):
            ctx = t0 < c.LC
            if ctx and skip_ctx:
                continue
            hk = KC // 2
            k.dma("sp", acc[:, 0:hk, 0:n], AHT[0:hk * 128, t0:t0 + n].rearrange("(kc p) t -> p kc t", p=128), acc, reads=[AHT], writes=[acc])
            k.dma("sp", acc[:, hk:KC, 0:n], AHT[hk * 128:KC * 128, t0:t0 + n].rearrange("(kc p) t -> p kc t", p=128), acc, reads=[AHT], parts=[acc])
            for ei, wb in enumerate(wbs):
                g_ = gT[gi % 2]; gi += 1
                k.dma("sp", g_[:, :, 0:n], GT[ei][0:Fe, t0:t0 + n].rearrange("(kc p) t -> p kc t", p=128), g_, reads=[GT[ei]], writes=[g_])
                for j in range(KC):
                    wt = wts[it % 4]; ps = pss[it % 4]; it += 1
                    k.dma("sp", wt[:, :, :], wb[j], wt, reads=[wb], writes=[wt])
                    self.ws_tile(wt, 0, g_, kcn, n, [ps])
                    k.op("dve", lambda e: e.scalar_tensor_tensor(out=acc[:, j, 0:n], in0=ps[:, 0:n], scalar=self.mod(l, 5, j, ctx),
                                                                 in1=acc[:, j, 0:n], op0=ALU.mult, op1=ALU.add),
                         reads=[ps, acc, self.modT], writes=[acc])
            k.dma("pool", RTout[0:hk * 128, t0:t0 + n].rearrange("(kc p) t -> p kc t", p=128), acc[:, 0:hk, 0:n], acc, reads=[acc], parts=[RTout])
            k.dma("pool", RTout[hk * 128:KC * 128, t0:t0 + n].rearrange("(kc p) t -> p kc t", p=128), acc[:, hk:KC, 0:n], acc, reads=[acc], parts=[RTout])
        k.release(m)

    def build_all(self, stop=None):
        k, c, I = self.k, self.c, self.I
        D, T = c.D, c.T
        dr = lambda n_, sh, dt=F32: k.dram(n_, sh, dt)
        RT0, RT1, RT2 = dr("RT0", [D, T]), dr("RT1", [D, T]), dr("RT2", [D, T])
        AHT, AHT2 = dr("AHT", [D, T]), dr("AHT2", [D, T])
        ZDT, XBCT, GLUT = dr("ZDT", [T, c.OFF_X]), dr("XBCT", [c.DXBC, T]), dr("GLUT", [c.DCF, T])
        XTM, YB, VT = dr("XTM", [T, c.DS]), dr("YB", [T, c.DS]), dr("VT", [c.DCF, T])
        BTM, BT, CT = dr("BTM", [T, c.G * 128], BF16), dr("BT", [c.G * 128, T], BF16), dr("CT", [c.G * 128, T], BF16)
        MT = dr("MT", [D, T], BF16)
        GT = [dr("GT0", [max(c.FD, c.FE), T], BF16)] + [dr("GT%d" % e, [c.FE, T], BF16) for e in range(1, c.NE if c.depth > 1 else 1)]
        self.S = dict(RT0=RT0, RT1=RT1, RT2=RT2, AHT=AHT, AHT2=AHT2, ZDT=ZDT, XBCT=XBCT, GLUT=GLUT, XTM=XTM, YB=YB, VT=VT, MT=MT, GT=GT[0])
        self.stage_mods()
        self.stage_vecs()
        self.stage_p0(RT0)
        for l in range(c.depth):
            last = l == c.depth - 1
            src = RT0 if l == 0 else RT2
            post = None if l == 0 else (self.vecs[l - 1], 2, 3)
            self.stage_inproj(l, src, post, AHT, ZDT, XBCT, GLUT)
            if stop == ("inproj", l):
                return
            self.stage_ssmprep(l, XBCT, XTM, BTM, BT, CT)
            self.stage_ssd(l, ZDT, XTM, BTM, BT, CT, YB, MT)
            self.stage_conf(l, GLUT, VT, MT, skip_ctx=last)
            if stop == ("mix", l):
                return
            self.stage_proj_res("wob%d" % l, l, I["w_out"][l], D, MT, 2, AHT, RT1, 1024 if c.TB >= 1024 else c.TB, skip_ctx=last)
            if stop == ("out", l):
                return
            j = l // 2
            if l % 2 == 0:
                self.stage_ffn_up(l, RT1, AHT2, GT, [(I["ffn_w1"][j], I["ffn_w3"][j], c.FD)], skip_ctx=last)
                self.stage_proj_res("w2b%d" % l, l, I["ffn_w2"][j], c.FD, GT[0], 5, AHT2, RT2, 512 if c.TB >= 512 else c.TB, skip_ctx=last)
            else:
                self.stage_ffn_up(l, RT1, AHT2, GT, [(I["moe_w1"][j, e], I["moe_w3"][j, e], c.FE) for e in range(c.NE)],
                                  skip_ctx=last, router=(I["router_w"][j], I["router_b"][j:j + 1, :]), nsub=min(256, c.NSUB), tb=min(512, c.TB))
                if stop == ("moeup", l):
                    return
                self.stage_moe_down(l, GT, [I["moe_w2"][j, e] for e in range(c.NE)], c.FE, AHT2, RT2, skip_ctx=last,
                                    tb=512 if c.TB >= 512 else c.TB)
            if stop == ("ffn", l):
                return
        self.stage_final(c.depth - 1, RT2)


class types_ns:
    pass


_RENAME = {"conv_w": "mamba_conv_w", "conv_b": "mamba_conv_b", "dtb_f": "dt_bias_fwd", "dtb_b": "dt_bias_bwd",
           "alog_f": "a_log_fwd", "alog_b": "a_log_bwd", "dskip": "d_skip", "cconv_w": "conf_conv_w",
           "cconv_b": "conf_conv_b", "cln_g": "conf_ln_g", "cln_b": "conf_ln_b"}
_SAME = ("ada_w", "ada_b", "w_in", "ssm_norm_w", "w_out", "ln1_g", "ln1_b", "ln2_g", "ln2_b", "ffn_w1", "ffn_w3",
         "ffn_w2", "router_w", "router_b", "moe_w1", "moe_w3", "moe_w2")
_PROGRAM = None


def _program():
    global _PROGRAM
    if _PROGRAM is None:
        nc = bass.Bass("TRN2", target_bir_lowering=False)
        mk = MK(nc, Cfg())
        mk.build_all()
        mk.k.finish([mk.out])
        mk.k.close()
        _PROGRAM = nc
    return _PROGRAM


def kernel(**inputs):
    g = lambda n: np.ascontiguousarray(np.asarray(inputs[n], dtype=np.float32))
    shared = {n: g(n) for n in _SAME}
    for kx, v in _RENAME.items():
        shared[kx] = g(v)
    x, c, ctx, c_ctx = g("x"), g("c"), g("ctx"), g("c_ctx")
    in_maps = []
    for b in range(N_CORES_USED):
        d = dict(shared)
        d["x"] = x[b]
        d["ctx"] = ctx[b]
        d["c2"] = np.stack([c[b], c_ctx], 0)
        in_maps.append(d)
    res = run_bass_kernel_spmd(_program(), in_maps, core_ids=list(range(N_CORES_USED)))
    return np.stack([res.results[b]["out"] for b in range(N_CORES_USED)], 0)
```
